# Optimizing a Trainium2 kernel written in Bass

```python
import math
import jax, jax.numpy as jnp
from jax import lax
import numpy as np

D_MODEL = 2048
BATCH = 8
SEQ = 4096
DEPTH = 4

N_MIXERS = 2
A_HEADS = 32
A_KV_HEADS = 4
A_HEAD_DIM = 64
WINDOW = 128
BLOCK = 128
A_QKV = (A_HEADS + 2 * A_KV_HEADS) * A_HEAD_DIM
REL_BUCKETS = 32
REL_MAX_DIST = 128
B_HEADS = 16
Q_LORA = 512
KV_LORA = 512
QK_NOPE = 128
QK_ROPE = 64
V_HEAD = 128
ROPE_THETA = 10000.0
B_DOWN = Q_LORA + KV_LORA + QK_ROPE
D_FF = 5632
N_EXPERTS = 8
TOP_K = 2
D_FF_EXPERT = 5632
N_MOD = 6
EPS = 1e-6
NEG_INF = -1e30

kernel_name = "hybrid_swa_sink_mla_moe_adaln"


def _rmsnorm(x, g):
    xf = x.astype(jnp.float32)
    y = xf * lax.rsqrt(jnp.mean(xf * xf, axis=-1, keepdims=True) + EPS)
    return (y * g.astype(jnp.float32)).astype(x.dtype)


def _modulate(h, shift, scale):
    return h * (1 + scale[:, None, :]) + shift[:, None, :]


def _t5_bucket(n):
    max_exact = REL_BUCKETS // 2
    nf = jnp.maximum(n, 1).astype(jnp.float32)
    large = max_exact + (jnp.log(nf / max_exact) / math.log(REL_MAX_DIST / max_exact)
                         * (REL_BUCKETS - max_exact)).astype(jnp.int32)
    large = jnp.minimum(large, REL_BUCKETS - 1)
    return jnp.where(n < max_exact, n, large)


def _rope(t, cos, sin):
    t1, t2 = jnp.split(t, 2, axis=-1)
    return jnp.concatenate([t1 * cos - t2 * sin, t1 * sin + t2 * cos], axis=-1).astype(t.dtype)


def _swa_mixer(h, wqkv, q_g, k_g, sinks, wo, rel_bias):
    B, S, _ = h.shape
    nb = S // BLOCK
    G = A_HEADS // A_KV_HEADS
    qkv = h @ wqkv
    q, k, v = jnp.split(qkv, [A_HEADS * A_HEAD_DIM, (A_HEADS + A_KV_HEADS) * A_HEAD_DIM], axis=-1)
    q = _rmsnorm(q.reshape(B, S, A_KV_HEADS, G, A_HEAD_DIM), q_g)
    k = _rmsnorm(k.reshape(B, S, A_KV_HEADS, A_HEAD_DIM), k_g)
    v = v.reshape(B, S, A_KV_HEADS, A_HEAD_DIM)
    qb = q.reshape(B, nb, BLOCK, A_KV_HEADS, G, A_HEAD_DIM).transpose(1, 0, 2, 3, 4, 5)

    def band(t):
        tb = t.reshape(B, nb, BLOCK, A_KV_HEADS, A_HEAD_DIM)
        prev = jnp.pad(tb, ((0, 0), (1, 0), (0, 0), (0, 0), (0, 0)))[:, :nb]
        return jnp.concatenate([prev, tb], axis=2).transpose(1, 0, 2, 3, 4)

    kb, vb = band(k), band(v)
    t_idx = jnp.arange(BLOCK)[:, None]
    j_idx = jnp.arange(2 * BLOCK)[None, :]
    dist = t_idx + BLOCK - j_idx
    in_window = (dist >= 0) & (dist < WINDOW)
    bias = rel_bias[_t5_bucket(jnp.maximum(dist, 0))]
    bias = bias.astype(jnp.float32).transpose(2, 0, 1).reshape(A_KV_HEADS, G, BLOCK, 2 * BLOCK)
    sink = sinks.astype(jnp.float32).reshape(A_KV_HEADS, G)[..., None, None]
    scale = A_HEAD_DIM ** -0.5

    def block_attn(args):
        blk, qi, ki, vi = args
        s = jnp.einsum('btkgd,bjkd->bkgtj', qi, ki).astype(jnp.float32) * scale + bias
        valid = in_window & (j_idx + blk * BLOCK >= BLOCK)
        s = jnp.where(valid, s, NEG_INF)
        m = jnp.maximum(jnp.max(s, axis=-1, keepdims=True), sink)
        p = jnp.exp(s - m)
        denom = jnp.sum(p, axis=-1, keepdims=True) + jnp.exp(sink - m)
        p = (p / denom).astype(vi.dtype)
        return jnp.einsum('bkgtj,bjkd->btkgd', p, vi)

    o = lax.map(block_attn, (jnp.arange(nb), qb, kb, vb))
    o = o.transpose(1, 0, 2, 3, 4, 5).reshape(B, S, A_HEADS * A_HEAD_DIM)
    return o @ wo


def _mla_mixer(h, wdown, q_a_g, kv_a_g, wuq, wukv, q_g, k_g, wo):
    B, S, _ = h.shape
    nb = S // BLOCK
    lat = h @ wdown
    cq, ckv, k_pe = jnp.split(lat, [Q_LORA, Q_LORA + KV_LORA], axis=-1)
    q = (_rmsnorm(cq, q_a_g) @ wuq).reshape(B, S, B_HEADS, QK_NOPE + QK_ROPE)
    kv = (_rmsnorm(ckv, kv_a_g) @ wukv).reshape(B, S, B_HEADS, QK_NOPE + V_HEAD)
    q_nope, q_pe = jnp.split(q, [QK_NOPE], axis=-1)
    k_nope, v = jnp.split(kv, [QK_NOPE], axis=-1)
    q_nope = _rmsnorm(q_nope, q_g[:QK_NOPE])
    q_pe = _rmsnorm(q_pe, q_g[QK_NOPE:])
    k_nope = _rmsnorm(k_nope, k_g[:QK_NOPE])
    k_pe = _rmsnorm(k_pe, k_g[QK_NOPE:])
    pos = jnp.arange(S, dtype=jnp.float32)
    inv = ROPE_THETA ** (-jnp.arange(0, QK_ROPE, 2, dtype=jnp.float32) / QK_ROPE)
    ang = pos[:, None] * inv[None, :]
    cos, sin = jnp.cos(ang), jnp.sin(ang)
    q_pe = _rope(q_pe, cos[:, None, :], sin[:, None, :])
    k_pe = _rope(k_pe, cos, sin)
    qn_b = q_nope.reshape(B, nb, BLOCK, B_HEADS, QK_NOPE).transpose(1, 0, 2, 3, 4)
    qp_b = q_pe.reshape(B, nb, BLOCK, B_HEADS, QK_ROPE).transpose(1, 0, 2, 3, 4)
    kpos = jnp.arange(S)
    scale = (QK_NOPE + QK_ROPE) ** -0.5

    def block_attn(args):
        blk, qn, qp = args
        qpos = blk * BLOCK + jnp.arange(BLOCK)
        s = (jnp.einsum('bthd,bshd->bhts', qn, k_nope).astype(jnp.float32)
             + jnp.einsum('bthr,bsr->bhts', qp, k_pe).astype(jnp.float32)) * scale
        s = jnp.where(kpos[None, :] <= qpos[:, None], s, NEG_INF)
        p = jax.nn.softmax(s, axis=-1).astype(v.dtype)
        return jnp.einsum('bhts,bshd->bthd', p, v)

    o = lax.map(block_attn, (jnp.arange(nb), qn_b, qp_b))
    o = o.transpose(1, 0, 2, 3, 4).reshape(B, S, B_HEADS * V_HEAD)
    return o @ wo


def _swiglu(h, wg, wu, wd):
    return (jax.nn.silu(h @ wg) * (h @ wu)) @ wd


def _moe(h, w_router, b_router, wg, wu, wd):
    B, S, D = h.shape
    t = h.reshape(B * S, D)
    logits = (t @ w_router).astype(jnp.float32) + b_router.astype(jnp.float32)
    top_val, top_idx = lax.top_k(logits, TOP_K)
    top_w = jax.nn.softmax(top_val, axis=-1)
    gates = jnp.sum(jax.nn.one_hot(top_idx, N_EXPERTS, dtype=jnp.float32) * top_w[..., None], axis=1)
    gates = gates.astype(h.dtype)
    out = jnp.zeros_like(t)
    for e in range(N_EXPERTS):
        out = out + gates[:, e:e + 1] * _swiglu(t, wg[e], wu[e], wd[e])
    return out.reshape(B, S, D)


def setup_inputs(seed: int = 0) -> dict:
    key = jax.random.key(seed)
    ks = iter(jax.random.split(key, 40))
    nA = (DEPTH + 1) // 2
    nB = DEPTH // 2

    def nrm(shape, fan_in, mult=1.0):
        return jax.random.normal(next(ks), shape, jnp.float32) * (mult * fan_in ** -0.5)

    def gain(shape):
        return 1.0 + 0.05 * jax.random.normal(next(ks), shape, jnp.float32)

    return {
        "x": jax.random.normal(next(ks), (BATCH, SEQ, D_MODEL), jnp.float32),
        "c": jax.random.normal(next(ks), (BATCH, D_MODEL), jnp.float32),
        "rel_bias": 0.5 * jax.random.normal(next(ks), (REL_BUCKETS, A_HEADS), jnp.float32),
        "ada_w": nrm((DEPTH, D_MODEL, N_MOD * D_MODEL), D_MODEL, 0.5),
        "ada_b": 0.02 * jax.random.normal(next(ks), (DEPTH, N_MOD * D_MODEL), jnp.float32),
        "norm_g": gain((DEPTH, 2, D_MODEL)),
        "a_wqkv": nrm((nA, D_MODEL, A_QKV), D_MODEL),
        "a_q_norm": gain((nA, A_HEAD_DIM)),
        "a_k_norm": gain((nA, A_HEAD_DIM)),
        "a_sinks": 0.5 * jax.random.normal(next(ks), (nA, A_HEADS), jnp.float32),
        "a_wo": nrm((nA, A_HEADS * A_HEAD_DIM, D_MODEL), A_HEADS * A_HEAD_DIM),
        "b_wdown": nrm((nB, D_MODEL, B_DOWN), D_MODEL),
        "b_q_a_norm": gain((nB, Q_LORA)),
        "b_kv_a_norm": gain((nB, KV_LORA)),
        "b_wuq": nrm((nB, Q_LORA, B_HEADS * (QK_NOPE + QK_ROPE)), Q_LORA),
        "b_wukv": nrm((nB, KV_LORA, B_HEADS * (QK_NOPE + V_HEAD)), KV_LORA),
        "b_q_norm": gain((nB, QK_NOPE + QK_ROPE)),
        "b_k_norm": gain((nB, QK_NOPE + QK_ROPE)),
        "b_wo": nrm((nB, B_HEADS * V_HEAD, D_MODEL), B_HEADS * V_HEAD),
        "ffn_wg": nrm((nA, D_MODEL, D_FF), D_MODEL),
        "ffn_wu": nrm((nA, D_MODEL, D_FF), D_MODEL),
        "ffn_wd": nrm((nA, D_FF, D_MODEL), D_FF),
        "moe_router": nrm((nB, D_MODEL, N_EXPERTS), D_MODEL),
        "moe_router_b": 0.01 * jax.random.normal(next(ks), (nB, N_EXPERTS), jnp.float32),
        "moe_wg": nrm((nB, N_EXPERTS, D_MODEL, D_FF_EXPERT), D_MODEL),
        "moe_wu": nrm((nB, N_EXPERTS, D_MODEL, D_FF_EXPERT), D_MODEL),
        "moe_wd": nrm((nB, N_EXPERTS, D_FF_EXPERT, D_MODEL), D_FF_EXPERT),
    }


def reference(x, c, rel_bias, ada_w, ada_b, norm_g,
              a_wqkv, a_q_norm, a_k_norm, a_sinks, a_wo,
              b_wdown, b_q_a_norm, b_kv_a_norm, b_wuq, b_wukv, b_q_norm, b_k_norm, b_wo,
              ffn_wg, ffn_wu, ffn_wd,
              moe_router, moe_router_b, moe_wg, moe_wu, moe_wd):
    c_act = jax.nn.silu(c)
    for layer in range(DEPTH):
        i = layer // N_MIXERS
        mod = c_act @ ada_w[layer] + ada_b[layer]
        shift1, scale1, gate1, shift2, scale2, gate2 = jnp.split(mod, N_MOD, axis=-1)
        h = _modulate(_rmsnorm(x, norm_g[layer, 0]), shift1, scale1)
        if layer % N_MIXERS == 0:
            y = _swa_mixer(h, a_wqkv[i], a_q_norm[i], a_k_norm[i], a_sinks[i], a_wo[i], rel_bias)
        else:
            y = _mla_mixer(h, b_wdown[i], b_q_a_norm[i], b_kv_a_norm[i], b_wuq[i], b_wukv[i],
                           b_q_norm[i], b_k_norm[i], b_wo[i])
        x = x + gate1[:, None, :] * y
        h = _modulate(_rmsnorm(x, norm_g[layer, 1]), shift2, scale2)
        if layer % 2 == 0:
            y = _swiglu(h, ffn_wg[i], ffn_wu[i], ffn_wd[i])
        else:
            y = _moe(h, moe_router[i], moe_router_b[i], moe_wg[i], moe_wu[i], moe_wd[i])
        x = x + gate2[:, None, :] * y
    return x
```

```python
import math
from contextlib import ExitStack

import numpy as np
import ml_dtypes

import concourse.bass as bass
import concourse.mybir as mybir
from concourse.bass_utils import run_bass_kernel_spmd

F32 = mybir.dt.float32
BF16 = mybir.dt.bfloat16
AF = mybir.ActivationFunctionType
ALU = mybir.AluOpType
AX = mybir.AxisListType

D = 2048
KD = D // 128
DEPTH = 4
A_HEADS, A_KV, A_HD = 32, 4, 64
A_QKV = (A_HEADS + 2 * A_KV) * A_HD
B_HEADS, Q_LORA, KV_LORA, QK_NOPE, QK_ROPE, V_HEAD = 16, 512, 512, 128, 64, 128
B_DOWN = Q_LORA + KV_LORA + QK_ROPE
D_FF = 5632
KF = D_FF // 128
N_EXP = 8
EPS = 1e-6
NEG = -30000.0
NT = 512
EPOCH = 50000


class Buf:
    __slots__ = ("name", "w", "r", "dsem", "dval")

    def __init__(self, name, fence=None):
        self.name = name
        self.w = dict(fence) if fence else {}
        self.r = {}
        self.dsem = None
        self.dval = 0


def _merge(dst, src):
    for k, (s, v) in src.items():
        if k not in dst or dst[k][1] < v:
            dst[k] = (s, v)


class Eng:
    def __init__(self, name, h, self_wait):
        self.name = name
        self.h = h
        self.self_wait = self_wait
        self.sems = []
        self.count = 0
        self.waited = {}


class K:
    def __init__(self, nc, stack):
        self.nc = nc
        self.stack = stack
        self.planning = False
        self.nsem = 0
        self.pe = Eng("pe", nc.tensor, False)
        self.act = Eng("act", nc.scalar, True)
        self.dve = Eng("dve", nc.vector, True)
        self.pool = Eng("pool", nc.gpsimd, True)
        self.sp = Eng("sp", nc.sync, False)
        self.engs = [self.pe, self.act, self.dve, self.pool, self.sp]
        self.scopes = []
        self.fence = {}
        self.free_dsems = []

    def new_sem(self, name):
        self.nsem += 1
        return self.stack.enter_context(self.nc.semaphore(f"{name}_{self.nsem}"))

    def _cur_sem(self, eng):
        if not eng.sems or eng.count >= EPOCH:
            eng.sems.append(self.new_sem("e" + eng.name))
            eng.count = 0
        return eng.sems[-1]

    def buf(self, name):
        if self.scopes:
            b = Buf(name, self.fence)
            self.scopes[-1]["bufs"].append(b)
        else:
            b = Buf(name)
        return b

    def open_scope(self):
        st = ExitStack()
        self.scopes.append({"bufs": [], "stack": st})
        return st

    def close_scope(self):
        sc = self.scopes.pop()
        for b in sc["bufs"]:
            _merge(self.fence, b.w)
            _merge(self.fence, b.r)
            if b.dsem is not None:
                self.free_dsems.append((b.dsem, b.dval))
                b.dsem = None
        sc["stack"].close()

    def _wait(self, eng, deps):
        for k, (s, v) in deps.items():
            own = any(s is es for es in eng.sems)
            if own and not eng.self_wait:
                continue
            if eng.waited.get(k, 0) >= v:
                continue
            eng.h.wait_ge(s, v)
            eng.waited[k] = v

    def _deps(self, reads, writes):
        deps = {}
        for b in reads:
            _merge(deps, b.w)
        for b in writes:
            _merge(deps, b.w)
            _merge(deps, b.r)
        return deps

    def op(self, eng, fn, reads=(), writes=()):
        if self.planning:
            return
        self._wait(eng, self._deps(reads, writes))
        sem = self._cur_sem(eng)
        ins = fn()
        ins.then_inc(sem, 1)
        eng.count += 1
        ev = {id(sem): (sem, eng.count)}
        for b in reads:
            _merge(b.r, ev)
        for b in writes:
            _merge(b.w, ev)

    def dma(self, q, out, in_, reads=(), writes=(), owner=None):
        if self.planning:
            return
        assert owner is not None
        if owner.dsem is None:
            if self.free_dsems:
                owner.dsem, owner.dval = self.free_dsems.pop()
            else:
                owner.dsem = self.new_sem("d" + owner.name[:8])
        deps = self._deps(reads, writes)
        if owner.dval:
            _merge(deps, {id(owner.dsem): (owner.dsem, owner.dval)})
        self._wait(q, deps)
        ins = q.h.dma_start(out=out, in_=in_)
        owner.dval += 16
        ins.then_inc(owner.dsem, 16)
        ev = {id(owner.dsem): (owner.dsem, owner.dval)}
        for b in reads:
            _merge(b.r, ev)
        for b in writes:
            _merge(b.w, ev)

    def finish(self, bufs):
        deps = {}
        for b in bufs:
            _merge(deps, b.w)
            _merge(deps, b.r)
        self._wait(self.sp, deps)


class WStream:
    def __init__(self, kk, slots, slot_bufs, depth):
        self.kk = kk
        self.slots = slots
        self.bufs = slot_bufs
        self.plan = []
        self.issued = 0
        self.taken = 0
        self.depth = depth

    def get(self, parts):
        kk = self.kk
        if kk.planning:
            self.plan.append(parts)
            return self.slots[0], self.bufs[0]
        i = self.taken
        self.taken += 1
        n = len(self.slots)
        while self.issued < min(len(self.plan), i + self.depth):
            j = self.issued
            sl, sb = self.slots[j % n], self.bufs[j % n]
            for (vf, src) in self.plan[j]:
                kk.dma(kk.pool, vf(sl), src, writes=[sb], owner=sb)
            self.issued += 1
        return self.slots[i % n], self.bufs[i % n]


SHAPES = {
    "rel_bias": [32, 32], "ada_w": [DEPTH, D, 6 * D], "ada_b": [DEPTH, 6 * D], "norm_g": [DEPTH, 2, D],
    "a_wqkv": [2, D, A_QKV], "a_q_norm": [2, A_HD], "a_k_norm": [2, A_HD], "a_sinks": [2, A_HEADS],
    "a_wo": [2, D, D], "b_wdown": [2, D, B_DOWN], "b_q_a_norm": [2, Q_LORA], "b_kv_a_norm": [2, KV_LORA],
    "b_wuq": [2, Q_LORA, B_HEADS * 192], "b_wukv": [2, KV_LORA, B_HEADS * 256], "b_q_norm": [2, 192],
    "b_k_norm": [2, 192], "b_wo": [2, D, D], "ffn_wg": [2, D, D_FF], "ffn_wu": [2, D, D_FF],
    "ffn_wd": [2, D_FF, D], "moe_router": [2, D, N_EXP], "moe_router_b": [2, N_EXP],
    "moe_wg": [2, N_EXP, D, D_FF], "moe_wu": [2, N_EXP, D, D_FF], "moe_wd": [2, N_EXP, D_FF, D],
}


class Prog:
    def __init__(self, S, layers, subs=("mix", "ffn")):
        self.S = S
        self.layers = layers
        self.subs = subs
        self.NTT = max(1, S // NT)
        self.nt = min(NT, S)
        self.nb = self.nt // 128
        self.used = []
        self.dbgb = []
        self.debug = False

    def tin(self, name):
        if name not in self.T:
            S = self.S
            cshape = {"x": ([S, D], F32), "cT": ([128, KD], F32), "c_ident": ([128, 128], F32),
                      "c_identb": ([128, 128], BF16), "c_antib": ([128, 128], BF16), "c_onesb": ([128, 128], BF16), "c_bd64": ([128, 128], BF16),
                      "c_trimask": ([128, 128], BF16), "c_rope": ([2, 64, S], BF16), "c_t5oh": ([32, 128], F32)}
            shp, ty = cshape[name] if name in cshape else (SHAPES[name], F32)
            self.T[name] = self.nc.dram_tensor(name, list(shp), ty, kind="ExternalInput").ap()
            self.used.append(name)
        return self.T[name]

    def build(self):
        nc = bass.Bass("TRN2", target_bir_lowering=False)
        self.nc = nc
        self.T = {}
        self.T["out"] = nc.dram_tensor("out", [self.S, D], F32, kind="ExternalOutput").ap()
        with ExitStack() as stack:
            stack.enter_context(nc.allow_non_contiguous_dma(reason="tiny constant loads"))
            self.stack = stack
            kk = K(nc, stack)
            self.kk = kk
            self._alloc_globals()
            kk.planning = True
            self._program()
            kk.planning = False
            self._program()
            kk.finish([b for t in self.xd for b in t] + self.dbgb)
        return nc

    def sb(self, name, shape, ty=F32):
        st = self.kk.scopes[-1]["stack"] if self.kk.scopes else self.stack
        self.nsb = getattr(self, "nsb", 0) + 1
        return st.enter_context(self.nc.sbuf_tensor(f"s{self.nsb}_{name}", list(shape), ty))

    def _alloc_globals(self):
        nc, kk = self.nc, self.kk
        self.psum = [self.stack.enter_context(nc.psum_tensor(f"ps{i}", [128, 512], F32)) for i in range(8)]
        self.pb = [kk.buf(f"ps{i}") for i in range(8)]
        self.wslots = [self.sb(f"wslot{i}", [128, 8192], BF16) for i in range(4)]
        wb = [kk.buf(f"wslot{i}") for i in range(4)]
        self.W = WStream(kk, [w[:, :] for w in self.wslots], wb, depth=3)
        self.mod = self.sb("mod", [128, 3, D])
        self.modb = [kk.buf(f"mod{i}") for i in range(3)]
        self.xp = [self.sb(f"xp{i}", [128, 512]) for i in range(4)]
        self.xpb = [kk.buf(f"xp{i}") for i in range(4)]
        self.ident = self.sb("ident", [128, 128])
        self.identb = self.sb("identb", [128, 128], BF16)
        self.antib = self.sb("antib", [128, 128], BF16)
        self.onesb = self.sb("onesb", [128, 128], BF16)
        self.bd64 = self.sb("bd64", [128, 128], BF16)
        self.trimask = self.sb("trimask", [128, 128], BF16)
        self.cT = self.sb("cT", [128, KD])
        self.epsc = self.sb("epsc", [128, 1])
        self.tophalf = self.sb("tophalf", [128, 128], BF16)
        self.ca = self.sb("ca", [128, KD, 128], BF16)
        self.cb = kk.buf("consts")
        self.xd = [[kk.buf(f"xd{t}_{i}") for i in range(self.nb * 4)] for t in range(self.NTT)]

    def dump(self, name, ap, reads):
        if self.kk.planning or not getattr(self, "debug", False):
            return
        t = self.nc.dram_tensor("dbg_" + name, list(ap.shape), ap.dtype, kind="ExternalOutput").ap()
        b = self.kk.buf("dbg_" + name)
        self.dbgb.append(b)
        self.kk.dma(self.kk.sp, t, ap, reads=reads, writes=[b], owner=b)

    def xdb(self, tt, b, cg=None):
        if cg is None:
            return self.xd[tt][b * 4:(b + 1) * 4]
        return [self.xd[tt][b * 4 + cg]]

    def next_ps(self, n=1):
        r = []
        for _ in range(n):
            r.append(self.ps_pool[self.psi % len(self.ps_pool)])
            self.psi += 1
        return r

    def _program(self):
        kk, nc = self.kk, self.nc
        self.W.taken = 0
        self.psi = 0
        self.xpi = 0
        self.ps_pool = list(range(8))
        kk.op(kk.dve, lambda: nc.vector.memset(self.epsc[:, :], EPS), writes=[self.cb])
        kk.op(kk.dve, lambda: nc.vector.memset(self.tophalf[0:64, :], 1.0), writes=[self.cb])
        kk.op(kk.dve, lambda: nc.vector.memset(self.tophalf[64:128, :], 0.0), writes=[self.cb])
        for dst, src in ((self.ident, "c_ident"), (self.identb, "c_identb"), (self.antib, "c_antib"), (self.onesb, "c_onesb"),
                         (self.bd64, "c_bd64"), (self.trimask, "c_trimask"), (self.cT, "cT")):
            kk.dma(kk.sp, dst[:, :], self.tin(src)[:, :], writes=[self.cb], owner=self.cb)
        kk.op(kk.act, lambda: nc.scalar.activation(out=self.cT[:, :], in_=self.cT[:, :], func=AF.Silu),
              reads=[self.cb], writes=[self.cb])
        kk.op(kk.dve, lambda: nc.vector.tensor_copy(
            out=self.ca[:, :, :], in_=self.cT[:, :].unsqueeze(2).to_broadcast([128, KD, 128])),
            reads=[self.cb], writes=[self.cb])
        first = True
        for layer in self.layers:
            i = layer // 2
            if "mix" in self.subs:
                src = self.tin("x") if first else self.T["out"]
                first = False
                if layer % 2 == 0:
                    self.swa_sublayer(layer, i, src)
                else:
                    self.mla_sublayer(layer, i, src)
            if "ffn" in self.subs:
                src = self.tin("x") if first else self.T["out"]
                first = False
                if layer % 2 == 0:
                    ws = [(self.tin("ffn_wg")[i], self.tin("ffn_wu")[i], self.tin("ffn_wd")[i])]
                    self.ffn_sublayer(layer, src, ws, moe=None)
                else:
                    ws = [(self.tin("moe_wg")[i, e], self.tin("moe_wu")[i, e], self.tin("moe_wd")[i, e])
                          for e in range(N_EXP)]
                    self.ffn_sublayer(layer, src, ws, moe=i)

    def mod_phase(self, layer, j):
        kk, nc = self.kk, self.nc
        aw = self.tin("ada_w")[layer]
        ab = self.tin("ada_b")
        kk.open_scope()
        tmps = [self.sb(f"modtmp{i}", [128, 512]) for i in range(2)]
        tbs = [kk.buf(f"modtmp{i}") for i in range(2)]
        gtile = self.sb("modg", [128, D])
        gb = kk.buf("modg")
        kk.dma(kk.sp, gtile[:, :], self.tin("norm_g")[layer, j, :].partition_broadcast(128), writes=[gb], owner=gb)
        n = 0
        for dsti in range(3):
            m = (1, 0, 2)[dsti]
            for cg in range(4):
                c0 = (3 * j + m) * D + cg * 512
                src = aw[:, c0:c0 + 512].rearrange("(k p) n -> p k n", p=128)
                slot, sbuf_ = self.W.get([(lambda s: s[:, 0:KD * 512].rearrange("p (k n) -> p k n", k=KD), src)])
                (pi,) = self.next_ps(1)
                sv = slot[:, 0:KD * 512].rearrange("p (k n) -> p k n", k=KD)
                ps = self.psum[pi]
                tmp, tb = tmps[n % 2], tbs[n % 2]
                n += 1

                def mm(ps=ps, sv=sv):
                    for k in range(KD):
                        ins = nc.tensor.matmul(ps[:, :], self.ca[:, k, :], sv[:, k, :], start=(k == 0), stop=(k == KD - 1))
                    return ins
                kk.op(kk.pe, mm, reads=[sbuf_, self.cb], writes=[self.pb[pi]])
                kk.dma(kk.sp, tmp[:, :], ab[layer, c0:c0 + 512].partition_broadcast(128), writes=[tb], owner=tb)
                dst = self.mod[:, dsti, cg * 512:(cg + 1) * 512]
                kk.op(kk.dve, lambda ps=ps, dst=dst, tmp=tmp: nc.vector.tensor_tensor(
                    out=dst, in0=ps[:, :], in1=tmp[:, :], op=ALU.add),
                    reads=[self.pb[pi], tb], writes=[self.modb[dsti]])
                if dsti == 0:
                    g = gtile[:, cg * 512:(cg + 1) * 512]
                    kk.op(kk.dve, lambda dst=dst, g=g: nc.vector.scalar_tensor_tensor(
                        out=dst, in0=dst, scalar=1.0, in1=g, op0=ALU.add, op1=ALU.mult),
                        reads=[gb], writes=[self.modb[0]])
        kk.close_scope()

    def alloc_prep(self):
        kk = self.kk
        P = {}
        P["XT"] = [self.sb(f"xt{i}", [128, D]) for i in range(2)]
        P["xtb"] = [kk.buf(f"xt{i}") for i in range(2)]
        P["T1"] = [self.sb(f"t1_{i}", [128, 512]) for i in range(2)]
        P["t1b"] = [kk.buf(f"t1_{i}") for i in range(2)]
        P["T2"] = [self.sb(f"t2_{i}", [128, 512], BF16) for i in range(2)]
        P["t2b"] = [kk.buf(f"t2_{i}") for i in range(2)]
        P["ssq"] = [self.sb(f"ssq{i}", [128, 4]) for i in range(2)]
        P["ssqb"] = [kk.buf(f"ssq{i}") for i in range(2)]
        P["junk"] = self.sb("junk", [128, D], BF16)
        P["junkb"] = kk.buf("junk")
        return P

    def prep_tile(self, src, tt, P, HT, htb):
        kk, nc = self.kk, self.nc
        junk = P["junk"]
        for b in range(self.nb):
            r0 = tt * self.nt + b * 128
            xs, xb = P["XT"][b % 2], P["xtb"][b % 2]
            ssq, sqb = P["ssq"][b % 2], P["ssqb"][b % 2]
            kk.dma(kk.sp, xs[:, :], src[r0:r0 + 128, :], reads=self.xdb(tt, b), writes=[xb], owner=xb)
            kk.op(kk.act, lambda xs=xs, ssq=ssq: nc.scalar.activation(
                out=junk[:, :], in_=xs[:, :], func=AF.Square, accum_out=ssq[:, 0:1]),
                reads=[xb], writes=[sqb, P["junkb"]])
            kk.op(kk.dve, lambda ssq=ssq: nc.vector.tensor_scalar(
                out=ssq[:, 1:2], in0=ssq[:, 0:1], scalar1=1.0 / D, scalar2=EPS, op0=ALU.mult, op1=ALU.add),
                reads=[sqb], writes=[sqb])
            kk.op(kk.act, lambda ssq=ssq: nc.scalar.activation(out=ssq[:, 2:3], in_=ssq[:, 1:2], func=AF.Sqrt),
                  reads=[sqb], writes=[sqb])
            kk.op(kk.dve, lambda ssq=ssq: nc.vector.reciprocal(out=ssq[:, 3:4], in_=ssq[:, 2:3]),
                  reads=[sqb], writes=[sqb])
            for cg in range(4):
                t1, tb_ = P["T1"][cg % 2], P["t1b"][cg % 2]
                cs = slice(cg * 512, (cg + 1) * 512)
                kk.op(kk.dve, lambda xs=xs, ssq=ssq, t1=t1, cs=cs: nc.vector.scalar_tensor_tensor(
                    out=t1[:, :], in0=xs[:, cs], scalar=ssq[:, 3:4], in1=self.mod[:, 0, cs], op0=ALU.mult, op1=ALU.mult),
                    reads=[xb, sqb, self.modb[0]], writes=[tb_])
                t2, t2b_ = P["T2"][cg % 2], P["t2b"][cg % 2]
                kk.op(kk.dve, lambda t1=t1, t2=t2, cs=cs: nc.vector.tensor_tensor(
                    out=t2[:, :], in0=t1[:, :], in1=self.mod[:, 1, cs], op=ALU.add),
                    reads=[self.modb[1], tb_], writes=[t2b_])
                (pi,) = self.next_ps(1)
                psb = self.psum[pi][:, :].bitcast(BF16)

                def tr(psb=psb, t2=t2):
                    for q in range(4):
                        ins = nc.tensor.transpose(psb[:, q * 128:(q + 1) * 128], t2[:, q * 128:(q + 1) * 128], self.identb[:, :])
                    return ins
                kk.op(kk.pe, tr, reads=[t2b_, self.cb], writes=[self.pb[pi]])
                dst = HT[:, cg * 4:(cg + 1) * 4, b * 128:(b + 1) * 128]
                kk.op(kk.act, lambda psb=psb, dst=dst: nc.scalar.copy(
                    out=dst, in_=psb[:, 0:512].rearrange("p (q t) -> p q t", q=4)),
                    reads=[self.pb[pi]], writes=[htb])

    def alloc_epi(self):
        kk = self.kk
        self.etmp = [self.sb(f"etmp{i}", [128, 512]) for i in range(2)]
        self.etmpb = [kk.buf(f"etmp{i}") for i in range(2)]

    def resid_add(self, tt, b, cg, src, ps, pbuf, tok_gate=None, tgb=None):
        kk, nc = self.kk, self.nc
        r0 = tt * self.nt + b * 128
        si = self.xpi % 4
        self.xpi += 1
        xp, xpb = self.xp[si], self.xpb[si]
        cs = slice(cg * 512, (cg + 1) * 512)
        xdb = self.xdb(tt, b, cg)
        kk.dma(kk.sp, xp[:, :], src[r0:r0 + 128, cs], reads=xdb, writes=[xpb], owner=xpb)
        tmp, tmpb = self.etmp[si % 2], self.etmpb[si % 2]
        if tok_gate is None:
            kk.op(kk.dve, lambda: nc.vector.tensor_tensor(out=tmp[:, :], in0=ps[:, :], in1=self.mod[:, 2, cs], op=ALU.mult),
                  reads=[pbuf, self.modb[2]], writes=[tmpb])
        else:
            kk.op(kk.dve, lambda: nc.vector.scalar_tensor_tensor(
                out=tmp[:, :], in0=ps[:, :], scalar=tok_gate, in1=self.mod[:, 2, cs], op0=ALU.mult, op1=ALU.mult),
                reads=[pbuf, self.modb[2], tgb], writes=[tmpb])
        kk.op(kk.dve, lambda: nc.vector.tensor_tensor(out=xp[:, :], in0=xp[:, :], in1=tmp[:, :], op=ALU.add),
              reads=[tmpb], writes=[xpb])
        kk.dma(kk.sp, self.T["out"][r0:r0 + 128, cs], xp[:, :], reads=[xpb], writes=xdb, owner=xpb)

    def ffn_sublayer(self, layer, src, weights, moe):
        kk, nc = self.kk, self.nc
        nt, nb = self.nt, self.nb
        self.mod_phase(layer, 1)
        kk.open_scope()
        P = self.alloc_prep()
        self.alloc_epi()
        HT = self.sb("HT", [128, KD, nt], BF16)
        htb = kk.buf("HT")
        ACT = self.sb("ACTT", [128, KF, nt], BF16)
        actb = kk.buf("ACTT")
        sil = [self.sb(f"sil{i}", [128, nt], BF16) for i in range(2)]
        silb = [kk.buf(f"sil{i}") for i in range(2)]
        if moe is not None:
            wr = self.sb("wr", [128, KD, N_EXP], BF16)
            wrb = kk.buf("wr")
            rb = self.sb("rb", [128, N_EXP])
            rbb = kk.buf("rb")
            gates = self.sb("gates", [128, nb, N_EXP])
            gtb = kk.buf("gates")
            rt = self.sb("rt", [128, 4, N_EXP])
            rtb = kk.buf("rt")
            kk.dma(kk.pool, wr[:, :, :], self.tin("moe_router")[moe].rearrange("(k p) e -> p k e", p=128),
                   writes=[wrb], owner=wrb)
            kk.dma(kk.sp, rb[:, :], self.tin("moe_router_b")[moe, :].partition_broadcast(128), writes=[rbb], owner=rbb)
        self.dump(f"mod{layer}", self.mod[:, :, :], self.modb)
        for tt in range(self.NTT):
            self.prep_tile(src, tt, P, HT, htb)
            if tt == 0:
                self.dump(f"HT{layer}", HT[:, :, :], [htb])
            if moe is not None:
                for b in range(nb):
                    (pi,) = self.next_ps(1)
                    ps = self.psum[pi]

                    def mm(ps=ps, b=b):
                        for k in range(KD):
                            ins = nc.tensor.matmul(ps[:, 0:N_EXP], HT[:, k, b * 128:(b + 1) * 128], wr[:, k, :],
                                                   start=(k == 0), stop=(k == KD - 1))
                        return ins
                    kk.op(kk.pe, mm, reads=[htb, wrb], writes=[self.pb[pi]])
                    lg, mx, ex, msk = rt[:, 0, :], rt[:, 1, :], rt[:, 2, :], rt[:, 3, :]
                    den = rt[:, 1, 2:3]
                    kk.op(kk.dve, lambda ps=ps: nc.vector.tensor_tensor(out=lg, in0=ps[:, 0:N_EXP], in1=rb[:, :], op=ALU.add),
                          reads=[self.pb[pi], rbb], writes=[rtb])
                    kk.op(kk.dve, lambda: nc.vector.max(out=mx, in_=lg), reads=[rtb], writes=[rtb])
                    kk.op(kk.dve, lambda: nc.vector.tensor_scalar(out=ex, in0=lg, scalar1=mx[:, 0:1], scalar2=None, op0=ALU.subtract),
                          reads=[rtb], writes=[rtb])
                    kk.op(kk.act, lambda: nc.scalar.activation(out=ex, in_=ex, func=AF.Exp), reads=[rtb], writes=[rtb])
                    kk.op(kk.dve, lambda: nc.vector.tensor_scalar(out=msk, in0=lg, scalar1=mx[:, 1:2], scalar2=None, op0=ALU.is_ge),
                          reads=[rtb], writes=[rtb])
                    kk.op(kk.dve, lambda: nc.vector.tensor_tensor(out=msk, in0=msk, in1=ex, op=ALU.mult), reads=[rtb], writes=[rtb])
                    kk.op(kk.dve, lambda: nc.vector.tensor_reduce(out=den, in_=msk, op=ALU.add, axis=AX.X), reads=[rtb], writes=[rtb])
                    kk.op(kk.dve, lambda: nc.vector.reciprocal(out=den, in_=den), reads=[rtb], writes=[rtb])
                    kk.op(kk.dve, lambda b=b: nc.vector.tensor_scalar(out=gates[:, b, :], in0=msk, scalar1=den, scalar2=None, op0=ALU.mult),
                          reads=[rtb], writes=[gtb])
            for e, (wg, wu, wd) in enumerate(weights):
                for fb in range(D_FF // 512):
                    views = []
                    for wmat in (wg, wu):
                        srcw = wmat[:, fb * 512:(fb + 1) * 512].rearrange("(k p) n -> p k n", p=128)
                        views.append(self.W.get([(lambda s: s[:, 0:KD * 512].rearrange("p (k n) -> p k n", k=KD), srcw)]))
                    for fc in range(4):
                        pg, pu = self.next_ps(2)
                        f = fb * 4 + fc
                        for (slot, sbuf_), pi in ((views[0], pg), (views[1], pu)):
                            sv = slot[:, 0:KD * 512].rearrange("p (k n) -> p k n", k=KD)
                            ps = self.psum[pi]

                            def mm(ps=ps, sv=sv, fc=fc):
                                for k in range(KD):
                                    ins = nc.tensor.matmul(ps[:, 0:nt], sv[:, k, fc * 128:(fc + 1) * 128], HT[:, k, :],
                                                           start=(k == 0), stop=(k == KD - 1))
                                return ins
                            kk.op(kk.pe, mm, reads=[sbuf_, htb], writes=[self.pb[pi]])
                        s_, sb_ = sil[f % 2], silb[f % 2]
                        kk.op(kk.act, lambda s_=s_, pg=pg: nc.scalar.activation(out=s_[:, :], in_=self.psum[pg][:, 0:nt], func=AF.Silu),
                              reads=[self.pb[pg]], writes=[sb_])
                        kk.op(kk.dve, lambda s_=s_, pu=pu, f=f: nc.vector.tensor_tensor(
                            out=ACT[:, f, :], in0=self.psum[pu][:, 0:nt], in1=s_[:, :], op=ALU.mult),
                            reads=[self.pb[pu], sb_], writes=[actb])
                if tt == 0 and e == 0:
                    self.dump(f"ACT{layer}", ACT[:, :, :], [actb])
                kslabs = [(0, 16), (16, 16), (32, 12)]
                for cg in range(4):
                    pis = self.next_ps(nb)
                    for (k0, kn) in kslabs:
                        srcw = wd[k0 * 128:(k0 + kn) * 128, cg * 512:(cg + 1) * 512].rearrange("(k p) n -> p k n", p=128)
                        slot, sbuf_ = self.W.get([(lambda s, kn=kn: s[:, 0:kn * 512].rearrange("p (k n) -> p k n", k=kn), srcw)])
                        sv = slot[:, 0:kn * 512].rearrange("p (k n) -> p k n", k=kn)
                        for b in range(nb):
                            ps = self.psum[pis[b]]

                            def mm(ps=ps, sv=sv, b=b, k0=k0, kn=kn):
                                for k in range(kn):
                                    ins = nc.tensor.matmul(ps[:, :], ACT[:, k0 + k, b * 128:(b + 1) * 128], sv[:, k, :],
                                                           start=(k0 + k == 0), stop=(k0 + k == KF - 1))
                                return ins
                            kk.op(kk.pe, mm, reads=[sbuf_, actb], writes=[self.pb[pis[b]]])
                    for b in range(nb):
                        srcx = src if e == 0 else self.T["out"]
                        if moe is None:
                            self.resid_add(tt, b, cg, srcx, self.psum[pis[b]], self.pb[pis[b]])
                        else:
                            self.resid_add(tt, b, cg, srcx, self.psum[pis[b]], self.pb[pis[b]],
                                           tok_gate=gates[:, b, e:e + 1], tgb=gtb)
        kk.close_scope()

    def swa_sublayer(self, layer, i, src):
        kk, nc = self.kk, self.nc
        nt, nb = self.nt, self.nb
        self.mod_phase(layer, 0)
        kk.open_scope()
        self.ps_pool = [2, 3, 4, 5, 6, 7]
        PO, PD = 0, 1
        wqkv = self.tin("a_wqkv")[i]
        wo = self.tin("a_wo")[i]
        P = self.alloc_prep()
        self.alloc_epi()
        HT = self.sb("HT", [128, KD, nt], BF16); htb = kk.buf("HT")
        KT = self.sb("KT", [128, 2, A_KV, (nb + 1) * 128], BF16); ktb = kk.buf("KT")
        VT = self.sb("VT", [128, nb + 1, 2, A_KV, 128], BF16); vtb = kk.buf("VT")
        hones = self.sb("hones", [128, 2, 128], BF16); honb = kk.buf("hones")
        kk.op(kk.dve, lambda: nc.vector.memset(KT[:, :, :, :], 0.0), writes=[ktb])
        kk.op(kk.dve, lambda: nc.vector.memset(VT[:, :, :, :, :], 0.0), writes=[vtb])
        kk.op(kk.dve, lambda: nc.vector.memset(hones[:, :, :], 0.0), writes=[honb])
        kk.op(kk.dve, lambda: nc.vector.memset(hones[:, 0, 0:64], 1.0), writes=[honb])
        kk.op(kk.dve, lambda: nc.vector.memset(hones[:, 1, 64:128], 1.0), writes=[honb])
        AO = self.sb("AO", [128, KD, nt], BF16); aob = kk.buf("AO")
        BT = [self.sb(f"BT{j}", [128, 2, 2, 256], BF16) for j in range(2)]; btb = [kk.buf(f"BT{j}") for j in range(2)]
        qT = [self.sb(f"qT{j}", [128, nt], BF16) for j in range(2)]; qtb = [kk.buf(f"qT{j}") for j in range(2)]
        PT = [self.sb(f"PT{j}", [128, 512], BF16) for j in range(4)]; ptb = [kk.buf(f"PT{j}") for j in range(4)]
        sq = [self.sb(f"sq{j}", [128, nt], BF16) for j in range(2)]; sqb = [kk.buf(f"sq{j}") for j in range(2)]
        rs = [self.sb(f"rs{j}", [128, nt]) for j in range(2)]; rsb = [kk.buf(f"rs{j}") for j in range(2)]
        gq = self.sb("gq", [128, 2]); gqb = kk.buf("gq")
        esk = self.sb("esk", [128, KD]); eskb = kk.buf("esk")
        bp = self.sb("bp", [32, 384], BF16); bpb = kk.buf("bp")
        rbt = self.sb("rbt", [32, 32 + 128]); rbtb = kk.buf("rbt")
        bpad = self.nc.dram_tensor(f"bpad{layer}_{int(kk.planning)}", [32, 384], BF16).ap()
        bpd = kk.buf("bpad")
        for hh in range(2):
            kk.dma(kk.sp, gq[hh * 64:(hh + 1) * 64, 0:1], self.tin("a_q_norm")[i, :].unsqueeze(1), writes=[gqb], owner=gqb)
            kk.dma(kk.sp, gq[hh * 64:(hh + 1) * 64, 1:2], self.tin("a_k_norm")[i, :].unsqueeze(1), writes=[gqb], owner=gqb)
            sk = self.tin("a_sinks")
            srcap = bass.AP(sk.tensor, i * A_HEADS + hh, [[0, 64], [2, KD]])
            kk.dma(kk.sp, esk[hh * 64:(hh + 1) * 64, :], srcap, writes=[eskb], owner=eskb)
        kk.op(kk.dve, lambda: nc.vector.tensor_scalar(out=gq[:, 0:1], in0=gq[:, 0:1], scalar1=A_HD ** -0.5, scalar2=None, op0=ALU.mult),
              reads=[], writes=[gqb])
        kk.op(kk.act, lambda: nc.scalar.activation(out=esk[:, :], in_=esk[:, :], func=AF.Exp), reads=[], writes=[eskb])
        kk.dma(kk.sp, rbt[:, 0:32], self.tin("rel_bias")[:, :], writes=[rbtb], owner=rbtb)
        kk.dma(kk.sp, rbt[:, 32:160], self.tin("c_t5oh")[:, :], writes=[rbtb], owner=rbtb)
        (pi,) = self.next_ps(1)
        rbt16 = self.sb("rbt16", [128, 256], BF16)
        kk.op(kk.dve, lambda: nc.vector.memset(rbt16[:, :], 0.0), writes=[rbtb])
        kk.op(kk.dve, lambda: nc.vector.tensor_copy(out=rbt16[0:32, 0:32], in_=rbt[:, 0:32]), reads=[rbtb], writes=[rbtb])
        kk.op(kk.dve, lambda: nc.vector.tensor_copy(out=rbt16[0:32, 128:256], in_=rbt[:, 32:160]), reads=[rbtb], writes=[rbtb])
        kk.op(kk.pe, lambda: nc.tensor.matmul(self.psum[pi][:, 0:128], rbt16[:, 0:128], rbt16[:, 128:256], start=True, stop=True),
              reads=[rbtb], writes=[self.pb[pi]])
        kk.op(kk.dve, lambda: nc.vector.memset(bp[:, :], NEG), writes=[bpb])
        kk.op(kk.dve, lambda: nc.vector.tensor_copy(out=bp[:, 127:255], in_=self.psum[pi][0:32, 0:128]),
              reads=[self.pb[pi]], writes=[bpb])
        kk.dma(kk.sp, bpad[:, :], bp[:, :], reads=[bpb], writes=[bpd], owner=bpb)

        def load_bias(c, slot):
            for hh in range(2):
                h = 2 * c + hh
                srcap = bass.AP(bpad.tensor, h * 384, [[1, 128], [0, 2], [1, 256]])
                kk.dma(kk.sp, BT[slot][:, hh, :, :], srcap, reads=[bpd], writes=[btb[slot]], owner=btb[slot])

        def qk_norm(ps, pbuf, gcol, dst, dstb, j):
            s_, sb_ = sq[j % 2], sqb[j % 2]
            r_, rb_ = rs[j % 2], rsb[j % 2]
            kk.op(kk.act, lambda: nc.scalar.activation(out=s_[:, :], in_=ps[:, 0:nt], func=AF.Square), reads=[pbuf], writes=[sb_])
            (p2,) = self.next_ps(1)
            ps2 = self.psum[p2]
            kk.op(kk.pe, lambda: nc.tensor.matmul(ps2[:, 0:nt], self.bd64[:, :], s_[:, :], start=True, stop=True),
                  reads=[sb_, self.cb], writes=[self.pb[p2]])
            kk.op(kk.act, lambda: nc.scalar.activation(out=r_[:, :], in_=ps2[:, 0:nt], func=AF.Ln, scale=1.0 / A_HD, bias=self.epsc[:, 0:1]),
                  reads=[self.pb[p2], self.cb], writes=[rb_])
            kk.op(kk.act, lambda: nc.scalar.activation(out=r_[:, :], in_=r_[:, :], func=AF.Exp, scale=-0.5), reads=[], writes=[rb_])
            if isinstance(dst, tuple):
                for hh in range(2):
                    hs = slice(hh * 64, (hh + 1) * 64)
                    kk.op(kk.dve, lambda: nc.vector.scalar_tensor_tensor(out=dst[hh], in0=ps[hs, 0:nt], scalar=gq[hs, gcol:gcol + 1],
                                                                         in1=r_[hs, :], op0=ALU.mult, op1=ALU.mult),
                          reads=[pbuf, rb_, gqb], writes=[dstb])
            else:
                kk.op(kk.dve, lambda: nc.vector.scalar_tensor_tensor(out=dst, in0=ps[:, 0:nt], scalar=gq[:, gcol:gcol + 1], in1=r_[:, :],
                                                                     op0=ALU.mult, op1=ALU.mult),
                      reads=[pbuf, rb_, gqb], writes=[dstb])

        npt = 0
        for tt in range(self.NTT):
            self.prep_tile(src, tt, P, HT, htb)
            kparts = []
            for j in range(A_KV):
                ksrc = wqkv[:, 2048 + j * A_HD:2048 + (j + 1) * A_HD].rearrange("(k p) d -> p k d", p=128)
                for u in range(2):
                    c0 = j * 128 + u * 64
                    kparts.append((lambda s, c0=c0: s[:, 0:KD * 512].rearrange("p (k n) -> p k n", k=KD)[:, :, c0:c0 + 64], ksrc))
            slot, sbuf_ = self.W.get(kparts)
            sv = slot[:, 0:KD * 512].rearrange("p (k n) -> p k n", k=KD)
            for j in range(A_KV):
                (pi,) = self.next_ps(1)
                ps = self.psum[pi]

                def mm(ps=ps, j=j):
                    for k in range(KD):
                        ins = nc.tensor.matmul(ps[:, 0:nt], sv[:, k, j * 128:(j + 1) * 128], HT[:, k, :],
                                               start=(k == 0), stop=(k == KD - 1))
                    return ins
                kk.op(kk.pe, mm, reads=[sbuf_, htb], writes=[self.pb[pi]])
                qk_norm(ps, self.pb[pi], 1, (KT[0:64, 0, j, 128:128 + nt], KT[64:128, 1, j, 128:128 + nt]), ktb, j)
            vsrc = wqkv[:, 2304:2560].rearrange("(k p) n -> p k n", p=128)
            vview = lambda s: s[:, 0:KD * 256].rearrange("p (k n) -> p k n", k=KD)
            slot, sbuf_ = self.W.get([(vview, vsrc)])
            svv = vview(slot)
            for b2 in range(nb // 2):
                (pi,) = self.next_ps(1)
                ps = self.psum[pi]

                def mm(ps=ps, b2=b2):
                    for bb in range(2):
                        b = b2 * 2 + bb
                        for k in range(KD):
                            ins = nc.tensor.matmul(ps[:, bb * 256:(bb + 1) * 256], HT[:, k, b * 128:(b + 1) * 128], svv[:, k, :],
                                                   start=(k == 0), stop=(k == KD - 1))
                    return ins
                kk.op(kk.pe, mm, reads=[sbuf_, htb], writes=[self.pb[pi]])
                for hh in range(2):
                    kk.op(kk.act, lambda ps=ps, b2=b2, hh=hh: nc.scalar.copy(
                        out=VT[:, 1 + 2 * b2:3 + 2 * b2, hh, :, hh * 64:(hh + 1) * 64],
                        in_=ps[:, :].rearrange("p (b j d) -> p b j d", b=2, j=A_KV)),
                        reads=[self.pb[pi]], writes=[vtb])
            for c in range(KD):
                g = c // 4
                if c % 4 == 0:
                    srcw = wqkv[:, c * 128:c * 128 + 512].rearrange("(k p) n -> p k n", p=128)
                    qslot, qsb = self.W.get([(lambda s: s[:, 0:KD * 512].rearrange("p (k n) -> p k n", k=KD), srcw)])
                    qsv = qslot[:, 0:KD * 512].rearrange("p (k n) -> p k n", k=KD)
                load_bias(c, c % 2)
                bt = BT[c % 2]
                (pq,) = self.next_ps(1)
                psq = self.psum[pq]

                def mmq(psq=psq, qsv=qsv, c=c):
                    for k in range(KD):
                        ins = nc.tensor.matmul(psq[:, 0:nt], qsv[:, k, (c % 4) * 128:(c % 4 + 1) * 128], HT[:, k, :],
                                               start=(k == 0), stop=(k == KD - 1))
                    return ins
                kk.op(kk.pe, mmq, reads=[qsb, htb], writes=[self.pb[pq]])
                q_, qb_ = qT[c % 2], qtb[c % 2]
                qk_norm(psq, self.pb[pq], 0, q_[:, :], qb_, c)
                for b2 in range(nb // 2):
                    pss = self.next_ps(2)
                    pts = []
                    for hh in range(2):
                        ps = self.psum[pss[hh]]

                        def mms(ps=ps, hh=hh, b2=b2, bt=bt, q_=q_, g=g, tt=tt):
                            nc.tensor.matmul(ps[:, :], self.antib[:, :], bt[:, hh, :, :].rearrange("p a n -> p (a n)"), start=True, stop=False)
                            for bb in range(2):
                                b = b2 * 2 + bb
                                for kbi in range(2):
                                    if kbi == 1 and tt == 0 and b == 0:
                                        continue
                                    kpos = 1 + b - kbi
                                    last = (bb == 1 and kbi == 1)
                                    ins = nc.tensor.matmul(ps[:, bb * 256 + kbi * 128:bb * 256 + (kbi + 1) * 128],
                                                           KT[:, hh, g, kpos * 128:(kpos + 1) * 128],
                                                           q_[:, b * 128:(b + 1) * 128],
                                                           start=False, stop=last)
                            return ins
                        kk.op(kk.pe, mms, reads=[btb[c % 2], ktb, qb_, self.cb], writes=[self.pb[pss[hh]]])
                        pt, ptb_ = PT[npt % 4], ptb[npt % 4]
                        npt += 1
                        kk.op(kk.act, lambda ps=ps, pt=pt: nc.scalar.activation(out=pt[:, :], in_=ps[:, :], func=AF.Exp),
                              reads=[self.pb[pss[hh]]], writes=[ptb_])
                        pts.append((pt, ptb_))

                    def mmo(b2=b2, pts=pts, g=g, tt=tt):
                        for (pso, lhs_fn) in ((self.psum[PO], lambda kpos, hh: VT[:, kpos, hh, g, :]),
                                              (self.psum[PD], lambda kpos, hh: hones[:, hh, :])):
                            for bb in range(2):
                                b = b2 * 2 + bb
                                terms = [(hh, kbi) for hh in range(2) for kbi in (1, 0)
                                         if not (kbi == 1 and tt == 0 and b == 0)]
                                for ti, (hh, kbi) in enumerate(terms):
                                    kpos = 1 + b - kbi
                                    ins = nc.tensor.matmul(pso[:, b * 128:(b + 1) * 128], lhs_fn(kpos, hh),
                                                           pts[hh][0][:, bb * 256 + kbi * 128:bb * 256 + (kbi + 1) * 128],
                                                           start=(ti == 0), stop=(ti == len(terms) - 1))
                        return ins
                    kk.op(kk.pe, mmo, reads=[pts[0][1], pts[1][1], vtb, honb], writes=[self.pb[PO], self.pb[PD]])
                r_, rb_ = rs[c % 2], rsb[c % 2]
                kk.op(kk.act, lambda r_=r_, c=c: nc.scalar.activation(out=r_[:, :], in_=self.psum[PD][:, 0:nt], func=AF.Identity,
                                                                      bias=esk[:, c:c + 1]),
                      reads=[self.pb[PD], eskb], writes=[rb_])
                kk.op(kk.dve, lambda r_=r_: nc.vector.reciprocal(out=r_[:, :], in_=r_[:, :]), reads=[], writes=[rb_])
                kk.op(kk.dve, lambda r_=r_, c=c: nc.vector.tensor_tensor(out=AO[:, c, :], in0=self.psum[PO][:, 0:nt], in1=r_[:, :], op=ALU.mult),
                      reads=[self.pb[PO], rb_], writes=[aob])
            for cg in range(4):
                srcw = wo[:, cg * 512:(cg + 1) * 512].rearrange("(k p) n -> p k n", p=128)
                slot, sbuf_ = self.W.get([(lambda s: s[:, 0:KD * 512].rearrange("p (k n) -> p k n", k=KD), srcw)])
                osv = slot[:, 0:KD * 512].rearrange("p (k n) -> p k n", k=KD)
                for b in range(nb):
                    (pi,) = self.next_ps(1)
                    ps = self.psum[pi]

                    def mm(ps=ps, osv=osv, b=b):
                        for k in range(KD):
                            ins = nc.tensor.matmul(ps[:, :], AO[:, k, b * 128:(b + 1) * 128], osv[:, k, :], start=(k == 0), stop=(k == KD - 1))
                        return ins
                    kk.op(kk.pe, mm, reads=[sbuf_, aob], writes=[self.pb[pi]])
                    self.resid_add(tt, b, cg, src, ps, self.pb[pi])
            if tt + 1 < self.NTT:
                for hh in range(2):
                    kk.op(kk.dve, lambda: nc.vector.tensor_copy(out=KT[:, hh, :, 0:128], in_=KT[:, hh, :, nb * 128:(nb + 1) * 128]),
                          reads=[], writes=[ktb])
                    kk.op(kk.dve, lambda: nc.vector.tensor_copy(out=VT[:, 0, hh, :, :], in_=VT[:, nb, hh, :, :]), reads=[], writes=[vtb])
        self.ps_pool = list(range(8))
        kk.close_scope()


    def mla_sublayer(self, layer, i, src):
        kk, nc = self.kk, self.nc
        nt, nb, S = self.nt, self.nb, self.S
        NQ = S // nt
        self.mod_phase(layer, 0)
        wdown, wuq, wukv, wo = self.tin("b_wdown")[i], self.tin("b_wuq")[i], self.tin("b_wukv")[i], self.tin("b_wo")[i]
        rope = self.tin("c_rope")
        LAT = self.nc.dram_tensor(f"lat{layer}_{int(kk.planning)}", [B_DOWN + 64, S], BF16).ap()
        AOT = self.nc.dram_tensor(f"aot{layer}_{int(kk.planning)}", [D, S], BF16).ap()
        latd = [kk.buf(f"latd{t}") for t in range(NQ)]
        aotd = [kk.buf(f"aotd{t}") for t in range(NQ)]
        sview = lambda s: s[:, 0:KD * 512].rearrange("p (k n) -> p k n", k=KD)

        def small(scope_tag):
            d = {}
            d["sq"] = [self.sb(f"sq{j}", [128, nt], BF16) for j in range(2)]; d["sqb"] = [kk.buf(f"sq{j}") for j in range(2)]
            d["rs"] = [self.sb(f"rs{j}", [128, nt]) for j in range(2)]; d["rsb"] = [kk.buf(f"rs{j}") for j in range(2)]
            d["rp"] = [self.sb(f"rp{j}", [64, 2, nt], BF16) for j in range(2)]; d["rpb"] = [kk.buf(f"rp{j}") for j in range(2)]
            d["tt"] = [self.sb(f"tt{j}", [64, nt]) for j in range(2)]; d["ttb"] = [kk.buf(f"tt{j}") for j in range(2)]
            d["n"] = 0
            return d

        def rstd_of(d, pss, pbufs, nparts, nfeat):
            j = d["n"]; d["n"] += 1
            r_, rb_ = d["rs"][j % 2], d["rsb"][j % 2]
            (p2,) = self.next_ps(1)
            ps2 = self.psum[p2]
            for ci, (ps, pbuf) in enumerate(zip(pss, pbufs)):
                s_, sb_ = d["sq"][(j + ci) % 2], d["sqb"][(j + ci) % 2]
                kk.op(kk.act, lambda: nc.scalar.activation(out=s_[:, :], in_=ps, func=AF.Square), reads=[pbuf], writes=[sb_])
                lhs_ones = self.onesb[:, :] if nparts == 128 else self.tophalf[:, :]
                kk.op(kk.pe, lambda: nc.tensor.matmul(ps2[:, 0:nt], lhs_ones, s_[:, :],
                                                      start=(ci == 0), stop=(ci == len(pss) - 1)),
                      reads=[sb_, self.cb], writes=[self.pb[p2]])
            kk.op(kk.act, lambda: nc.scalar.activation(out=r_[0:nparts, :], in_=ps2[0:nparts, 0:nt], func=AF.Ln, scale=1.0 / nfeat,
                                                       bias=self.epsc[0:nparts, 0:1]),
                  reads=[self.pb[p2], self.cb], writes=[rb_])
            kk.op(kk.act, lambda: nc.scalar.activation(out=r_[0:nparts, :], in_=r_[0:nparts, :], func=AF.Exp, scale=-0.5), reads=[], writes=[rb_])
            return r_, rb_

        def rope_norm(d, psA, pbA, psB, pbB, gc, gs, gbuf, col0, dst, dstb):
            j = d["n"]
            rp, rpb = d["rp"][j % 2], d["rpb"][j % 2]
            kk.dma(kk.sp, rp[:, :, :], rope[:, :, col0:col0 + nt].rearrange("a p n -> p a n"), writes=[rpb], owner=rpb)
            r_, rb_ = rstd_of(d, [psA[:, 0:nt]], [pbA], 64, 64)
            t1, t1b = d["tt"][0], d["ttb"][0]
            t2, t2b = d["tt"][1], d["ttb"][1]
            kk.op(kk.dve, lambda: nc.vector.scalar_tensor_tensor(out=t1[:, :], in0=psA[0:64, 0:nt], scalar=gc, in1=rp[:, 0, :], op0=ALU.mult, op1=ALU.mult),
                  reads=[pbA, rpb, gbuf], writes=[t1b])
            kk.op(kk.dve, lambda: nc.vector.scalar_tensor_tensor(out=t2[:, :], in0=psB[0:64, 0:nt], scalar=gs, in1=rp[:, 1, :], op0=ALU.mult, op1=ALU.mult),
                  reads=[pbB, rpb, gbuf], writes=[t2b])
            kk.op(kk.dve, lambda: nc.vector.tensor_tensor(out=t1[:, :], in0=t1[:, :], in1=t2[:, :], op=ALU.add), reads=[t2b], writes=[t1b])
            kk.op(kk.dve, lambda: nc.vector.tensor_tensor(out=dst, in0=t1[:, :], in1=r_[0:64, :], op=ALU.mult), reads=[t1b, rb_], writes=[dstb])

        def load_gain(dst, name, off, n, cols=1):
            v = self.tin(name)
            ap = bass.AP(v.tensor, i * v.shape[1] + off, [[1, n], [128, cols]])
            kk.dma(kk.sp, dst, ap, writes=[gnb], owner=gnb)

        kk.open_scope()
        gn = self.sb("gn", [128, 16]); gnb = kk.buf("gn")
        load_gain(gn[:, 0:4], "b_q_a_norm", 0, 128, 4)
        load_gain(gn[:, 4:8], "b_kv_a_norm", 0, 128, 4)
        load_gain(gn[0:64, 8:9], "b_k_norm", 128, 64)
        load_gain(gn[0:32, 9:10], "b_k_norm", 160, 32)
        load_gain(gn[32:64, 9:10], "b_k_norm", 128, 32)
        P = self.alloc_prep()
        d = small("p1")
        HT = self.sb("HT", [128, KD, nt], BF16); htb = kk.buf("HT")
        stg = [self.sb(f"stg{j}", [128, nt], BF16) for j in range(4)]; stgb = [kk.buf(f"stg{j}") for j in range(4)]
        zt = self.sb("zt", [64, nt], BF16); ztb = kk.buf("zt")
        kk.op(kk.dve, lambda: nc.vector.memset(zt[:, :], 0.0), writes=[ztb])
        nst = 0
        for tt in range(NQ):
            self.prep_tile(src, tt, P, HT, htb)
            cols = slice(tt * nt, (tt + 1) * nt)
            for half in range(2):
                srcw = wdown[:, half * 512:(half + 1) * 512].rearrange("(k p) n -> p k n", p=128)
                slot, sbuf_ = self.W.get([(sview, srcw)])
                sv = sview(slot)
                pis = self.next_ps(4)
                for cc in range(4):
                    ps = self.psum[pis[cc]]

                    def mm():
                        for k in range(KD):
                            ins = nc.tensor.matmul(ps[:, 0:nt], sv[:, k, cc * 128:(cc + 1) * 128], HT[:, k, :], start=(k == 0), stop=(k == KD - 1))
                        return ins
                    kk.op(kk.pe, mm, reads=[sbuf_, htb], writes=[self.pb[pis[cc]]])
                r_, rb_ = rstd_of(d, [self.psum[p][:, 0:nt] for p in pis], [self.pb[p] for p in pis], 128, 512)
                for cc in range(4):
                    st, stb = stg[nst % 4], stgb[nst % 4]
                    nst += 1
                    kk.op(kk.dve, lambda: nc.vector.scalar_tensor_tensor(
                        out=st[:, :], in0=self.psum[pis[cc]][:, 0:nt], scalar=gn[:, half * 4 + cc:half * 4 + cc + 1], in1=r_[:, :],
                        op0=ALU.mult, op1=ALU.mult), reads=[self.pb[pis[cc]], rb_, gnb], writes=[stb])
                    r0 = half * 512 + cc * 128
                    kk.dma(kk.sp, LAT[r0:r0 + 128, cols], st[:, :], reads=[stb], writes=[latd[tt]], owner=stb)
            v256 = lambda s: s[:, 0:KD * 256].rearrange("p (k n) -> p k n", k=KD)
            wsl = lambda a, n: wdown[:, 1024 + a:1024 + a + n].rearrange("(k p) n -> p k n", p=128)
            parts = []
            for (c0, a, n) in ((0, 0, 64), (64, 0, 64), (128, 32, 32), (160, 0, 32), (192, 32, 32), (224, 0, 32)):
                parts.append((lambda s, c0=c0, n=n: v256(s)[:, :, c0:c0 + n], wsl(a, n)))
            slot, sbuf_ = self.W.get(parts)
            sv = v256(slot)
            pA, pB = self.next_ps(2)
            psA, psB = self.psum[pA], self.psum[pB]

            def mmk():
                for k in range(KD):
                    nc.tensor.matmul(psA[:, 0:nt], sv[:, k, 0:128], HT[:, k, :], start=(k == 0), stop=(k == KD - 1))
                for k in range(KD):
                    ins = nc.tensor.matmul(psB[:, 0:nt], sv[:, k, 128:256], HT[:, k, :], start=(k == 0), stop=(k == KD - 1))
                return ins
            kk.op(kk.pe, mmk, reads=[sbuf_, htb], writes=[self.pb[pA], self.pb[pB]])
            st, stb = stg[nst % 4], stgb[nst % 4]
            nst += 1
            rope_norm(d, psA, self.pb[pA], psB, self.pb[pB], gn[0:64, 8:9], gn[0:64, 9:10], gnb, tt * nt, st[0:64, :], stb)
            kk.dma(kk.sp, LAT[1024:1088, cols], st[0:64, :], reads=[stb], writes=[latd[tt]], owner=stb)
            kk.dma(kk.sp, LAT[1088:1152, cols], zt[:, :], reads=[ztb], writes=[latd[tt]], owner=ztb)
        kk.close_scope()

        kk.open_scope()
        self.ps_pool = [2, 3, 4, 5, 6, 7]
        PO, PD = 0, 1
        gn = self.sb("gn", [128, 16]); gnb = kk.buf("gn")
        load_gain(gn[:, 0:1], "b_q_norm", 0, 128)
        load_gain(gn[0:64, 1:2], "b_q_norm", 128, 64)
        load_gain(gn[0:32, 2:3], "b_q_norm", 160, 32)
        load_gain(gn[32:64, 2:3], "b_q_norm", 128, 32)
        load_gain(gn[:, 3:4], "b_k_norm", 0, 128)
        kk.op(kk.dve, lambda: nc.vector.tensor_scalar(out=gn[:, 0:1], in0=gn[:, 0:1], scalar1=192.0 ** -0.5, scalar2=None, op0=ALU.mult),
              reads=[], writes=[gnb])
        kk.op(kk.dve, lambda: nc.vector.tensor_scalar(out=gn[0:64, 1:3], in0=gn[0:64, 1:3], scalar1=192.0 ** -0.5, scalar2=None, op0=ALU.mult),
              reads=[], writes=[gnb])
        d = small("p2")
        KPE = self.sb("KPE", [128, S], BF16); kpeb = kk.buf("KPE")
        QN = self.sb("QN", [128, S], BF16); qnb = kk.buf("QN")
        QPE = self.sb("QPE", [128, S], BF16); qpeb = kk.buf("QPE")
        KN = self.sb("KN", [128, S], BF16); knb = kk.buf("KN")
        VH = self.sb("VH", [128, S // 128, 128], BF16); vhb = kk.buf("VH")
        LT = [self.sb(f"LT{j}", [128, 8, nt], BF16) for j in range(2)]; ltb = [kk.buf(f"LT{j}") for j in range(2)]
        PT = [self.sb(f"PT{j}", [128, 512], BF16) for j in range(3)]; ptb = [kk.buf(f"PT{j}") for j in range(3)]
        aos = [self.sb(f"aos{j}", [128, nt], BF16) for j in range(2)]; aosb = [kk.buf(f"aos{j}") for j in range(2)]
        rec = [self.sb(f"rec{j}", [128, nt]) for j in range(2)]; recb = [kk.buf(f"rec{j}") for j in range(2)]
        kk.dma(kk.sp, KPE[:, :], LAT[1024:1152, :], reads=latd, writes=[kpeb], owner=kpeb)
        kk.op(kk.dve, lambda: nc.vector.memset(QPE[64:128, :], 0.0), writes=[qpeb])
        nlt = npt = nao = 0
        for h in range(B_HEADS):
            vq = lambda s: s[:, 0:4 * 384].rearrange("p (k n) -> p k n", k=4)
            vkv = lambda s: s[:, 2048:2048 + 4 * 256].rearrange("p (k n) -> p k n", k=4)
            qsl = lambda a, n: wuq[:, h * 192 + a:h * 192 + a + n].rearrange("(k p) n -> p k n", p=128)
            parts = [(vkv, wukv[:, h * 256:(h + 1) * 256].rearrange("(k p) n -> p k n", p=128))]
            for (c0, a, n) in ((0, 0, 128), (128, 128, 64), (192, 128, 64), (256, 160, 32), (288, 128, 32), (320, 160, 32), (352, 128, 32)):
                parts.append((lambda s, c0=c0, n=n: vq(s)[:, :, c0:c0 + n], qsl(a, n)))
            slot, sbuf_ = self.W.get(parts)
            sq_, skv = vq(slot), vkv(slot)
            for tt in range(NQ):
                cols = slice(tt * nt, (tt + 1) * nt)
                lt, ltb_ = LT[nlt % 2], ltb[nlt % 2]
                nlt += 1
                kk.dma(kk.sp, lt[:, :, :], LAT[0:1024, cols].rearrange("(k p) n -> p k n", p=128), reads=[latd[tt]], writes=[ltb_], owner=ltb_)
                pqn, pqa, pqb, pkn = self.next_ps(4)

                def mmp():
                    for k in range(4):
                        nc.tensor.matmul(self.psum[pqn][:, 0:nt], sq_[:, k, 0:128], lt[:, k, :], start=(k == 0), stop=(k == 3))
                    for k in range(4):
                        nc.tensor.matmul(self.psum[pqa][:, 0:nt], sq_[:, k, 128:256], lt[:, k, :], start=(k == 0), stop=(k == 3))
                    for k in range(4):
                        nc.tensor.matmul(self.psum[pqb][:, 0:nt], sq_[:, k, 256:384], lt[:, k, :], start=(k == 0), stop=(k == 3))
                    for k in range(4):
                        ins = nc.tensor.matmul(self.psum[pkn][:, 0:nt], skv[:, k, 0:128], lt[:, 4 + k, :], start=(k == 0), stop=(k == 3))
                    return ins
                kk.op(kk.pe, mmp, reads=[sbuf_, ltb_], writes=[self.pb[p] for p in (pqn, pqa, pqb, pkn)])
                r_, rb_ = rstd_of(d, [self.psum[pqn][:, 0:nt]], [self.pb[pqn]], 128, 128)
                kk.op(kk.dve, lambda: nc.vector.scalar_tensor_tensor(out=QN[:, cols], in0=self.psum[pqn][:, 0:nt], scalar=gn[:, 0:1], in1=r_[:, :],
                                                                     op0=ALU.mult, op1=ALU.mult),
                      reads=[self.pb[pqn], rb_, gnb], writes=[qnb])
                r_, rb_ = rstd_of(d, [self.psum[pkn][:, 0:nt]], [self.pb[pkn]], 128, 128)
                kk.op(kk.dve, lambda: nc.vector.scalar_tensor_tensor(out=KN[:, cols], in0=self.psum[pkn][:, 0:nt], scalar=gn[:, 3:4], in1=r_[:, :],
                                                                     op0=ALU.mult, op1=ALU.mult),
                      reads=[self.pb[pkn], rb_, gnb], writes=[knb])
                rope_norm(d, self.psum[pqa], self.pb[pqa], self.psum[pqb], self.pb[pqb], gn[0:64, 1:2], gn[0:64, 2:3], gnb,
                          tt * nt, QPE[0:64, cols], qpeb)
                (pv,) = self.next_ps(1)

                def mmv():
                    for b in range(nb):
                        for k in range(4):
                            ins = nc.tensor.matmul(self.psum[pv][:, b * 128:(b + 1) * 128], lt[:, 4 + k, b * 128:(b + 1) * 128], skv[:, k, 128:256],
                                                   start=(k == 0), stop=(k == 3))
                    return ins
                kk.op(kk.pe, mmv, reads=[sbuf_, ltb_], writes=[self.pb[pv]])
                kk.op(kk.act, lambda: nc.scalar.copy(out=VH[:, tt * nb:(tt + 1) * nb, :],
                                                     in_=self.psum[pv][:, 0:nt].rearrange("p (b n) -> p b n", b=nb)),
                      reads=[self.pb[pv]], writes=[vhb])
            for qt in range(NQ):
                nkb = (qt + 1) * nb
                for j in range(nkb):
                    r = j - qt * nb
                    c0 = max(0, r) * 128
                    qc = slice(qt * nt + c0, (qt + 1) * nt)
                    kc = slice(j * 128, (j + 1) * 128)
                    (pi,) = self.next_ps(1)
                    ps = self.psum[pi]

                    def mms():
                        nc.tensor.matmul(ps[:, c0:nt], KN[:, kc], QN[:, qc], start=True, stop=False)
                        ins = nc.tensor.matmul(ps[:, c0:nt], KPE[:, kc], QPE[:, qc], start=False, stop=(r < 0))
                        if r >= 0:
                            ins = nc.tensor.matmul(ps[:, c0:c0 + 128], self.identb[:, :], self.trimask[:, :], start=False, stop=True)
                        return ins
                    kk.op(kk.pe, mms, reads=[knb, qnb, kpeb, qpeb, self.cb], writes=[self.pb[pi]])
                    pt, ptb_ = PT[npt % 3], ptb[npt % 3]
                    npt += 1
                    kk.op(kk.act, lambda: nc.scalar.activation(out=pt[:, c0:nt], in_=ps[:, c0:nt], func=AF.Exp), reads=[self.pb[pi]], writes=[ptb_])

                    def mmo():
                        nc.tensor.matmul(self.psum[PO][:, c0:nt], VH[:, j, :], pt[:, c0:nt], start=(j == 0), stop=(j == nkb - 1))
                        return nc.tensor.matmul(self.psum[PD][:, c0:nt], self.onesb[:, :], pt[:, c0:nt], start=(j == 0), stop=(j == nkb - 1))
                    kk.op(kk.pe, mmo, reads=[ptb_, vhb, self.cb], writes=[self.pb[PO], self.pb[PD]])
                rc, rcb = rec[nao % 2], recb[nao % 2]
                ao, aob_ = aos[nao % 2], aosb[nao % 2]
                nao += 1
                kk.op(kk.dve, lambda: nc.vector.reciprocal(out=rc[:, :], in_=self.psum[PD][:, 0:nt]), reads=[self.pb[PD]], writes=[rcb])
                kk.op(kk.dve, lambda: nc.vector.tensor_tensor(out=ao[:, :], in0=self.psum[PO][:, 0:nt], in1=rc[:, :], op=ALU.mult),
                      reads=[self.pb[PO], rcb], writes=[aob_])
                kk.dma(kk.sp, AOT[h * 128:(h + 1) * 128, qt * nt:(qt + 1) * nt], ao[:, :], reads=[aob_], writes=[aotd[qt]], owner=aob_)
        self.ps_pool = list(range(8))
        kk.close_scope()

        kk.open_scope()
        self.alloc_epi()
        AI = [self.sb(f"AI{j}", [128, KD, nt], BF16) for j in range(2)]; aib = [kk.buf(f"AI{j}") for j in range(2)]
        for tt in range(NQ):
            ai, aib_ = AI[tt % 2], aib[tt % 2]
            kk.dma(kk.sp, ai[:, :, :], AOT[:, tt * nt:(tt + 1) * nt].rearrange("(k p) n -> p k n", p=128), reads=[aotd[tt]], writes=[aib_], owner=aib_)
            for cg in range(4):
                srcw = wo[:, cg * 512:(cg + 1) * 512].rearrange("(k p) n -> p k n", p=128)
                slot, sbuf_ = self.W.get([(sview, srcw)])
                osv = sview(slot)
                for b in range(nb):
                    (pi,) = self.next_ps(1)
                    ps = self.psum[pi]

                    def mm():
                        for k in range(KD):
                            ins = nc.tensor.matmul(ps[:, :], ai[:, k, b * 128:(b + 1) * 128], osv[:, k, :], start=(k == 0), stop=(k == KD - 1))
                        return ins
                    kk.op(kk.pe, mm, reads=[sbuf_, aib_], writes=[self.pb[pi]])
                    self.resid_add(tt, b, cg, src, ps, self.pb[pi])
        kk.close_scope()


def _consts(S):
    c = {}
    c["c_ident"] = np.eye(128, dtype=np.float32)
    c["c_identb"] = np.eye(128, dtype=np.float32).astype(ml_dtypes.bfloat16)
    c["c_antib"] = np.ascontiguousarray(np.eye(128, dtype=np.float32)[::-1]).astype(ml_dtypes.bfloat16)
    c["c_onesb"] = np.ones((128, 128), dtype=np.float32).astype(ml_dtypes.bfloat16)
    bd = np.zeros((128, 128), dtype=np.float32)
    bd[:64, :64] = 1.0
    bd[64:, 64:] = 1.0
    c["c_bd64"] = bd.astype(ml_dtypes.bfloat16)
    s = np.arange(128)[:, None]
    t = np.arange(128)[None, :]
    c["c_trimask"] = np.where(t >= s, 0.0, NEG).astype(np.float32).astype(ml_dtypes.bfloat16)
    pos = np.arange(S, dtype=np.float32)
    inv = (10000.0 ** (-np.arange(0, 64, 2, dtype=np.float32) / 64)).astype(np.float32)
    ang = pos[None, :] * inv[:, None]
    cos = np.cos(ang).astype(np.float32)
    sin = np.sin(ang).astype(np.float32)
    rope = np.stack([np.concatenate([cos, cos], 0), np.concatenate([-sin, sin], 0)], 0)
    c["c_rope"] = rope.astype(ml_dtypes.bfloat16)
    n = np.arange(128)
    max_exact = 16
    nf = np.maximum(n, 1).astype(np.float32)
    large = max_exact + (np.log(nf / max_exact) / math.log(128 / max_exact) * (32 - max_exact)).astype(np.int32)
    large = np.minimum(large, 31)
    bucket = np.where(n < max_exact, n, large)
    oh = np.zeros((32, 128), dtype=np.float32)
    oh[bucket, n] = 1.0
    c["c_t5oh"] = oh
    return c


_PROG_CACHE = {}


def run_prog(inputs, S, layers, subs, n_cores, trace=False, debug=False):
    key = (S, tuple(layers), tuple(subs))
    if key not in _PROG_CACHE:
        p = Prog(S, layers, subs)
        p.debug = debug
        _PROG_CACHE[key] = (p.build(), p.used)
    nc, used = _PROG_CACHE[key]
    consts = _consts(S)
    shared = {}
    for k in used:
        if k in consts:
            shared[k] = consts[k]
        elif k not in ("x", "cT"):
            shared[k] = np.ascontiguousarray(inputs[k], dtype=np.float32)
    in_maps = []
    for b in range(n_cores):
        m = dict(shared)
        m["x"] = np.ascontiguousarray(inputs["x"][b, :S], dtype=np.float32)
        m["cT"] = np.ascontiguousarray(np.asarray(inputs["c"][b], dtype=np.float32).reshape(KD, 128).T)
        in_maps.append(m)
    res = run_bass_kernel_spmd(nc, in_maps, core_ids=list(range(n_cores)), trace=trace)
    out = np.stack([r["out"] for r in res.results], 0)
    if trace or debug:
        return out, res
    return out


def kernel(**inputs):
    inputs = {k: np.asarray(v) for k, v in inputs.items()}
    B, S, _ = inputs["x"].shape
    out = run_prog(inputs, S, list(range(DEPTH)), ("mix", "ffn"), B)
    return out.astype(np.float32)
```
